# Optimizing a Trainium2 kernel written in Bass

```python
import math
import jax, jax.numpy as jnp
from jax import lax
import numpy as np

D_MODEL = 1024
BATCH = 1
SEQ = 16384
DEPTH = 1

D_MIX = D_MODEL
HEAD_DIM = 64
RET_WIDTH = D_MIX // 2
SB_WIDTH = D_MIX - RET_WIDTH
N_RET_HEADS = RET_WIDTH // HEAD_DIM
N_SB_HEADS = SB_WIDTH // HEAD_DIM
RET_CHUNK = 128
SB_BLOCK = 128
ROPE_BASE = 10000.0
IN_COLS = 4 * RET_WIDTH + 3 * SB_WIDTH
N_KEYS = 128
N_EXPERTS = N_KEYS * N_KEYS
PEER_HEADS = 8
PEER_TOPK = 16
PEER_QDIM = 256
PEER_HALF = PEER_QDIM // 2
PEER_BLOCK = 128
EPS = 1e-6

kernel_name = "hymba_retention_stickbreak_peer_adaln"


def rms_norm(x, gain):
    xf = x.astype(jnp.float32)
    y = xf * lax.rsqrt(jnp.mean(xf * xf, axis=-1, keepdims=True) + EPS)
    return (y * gain.astype(jnp.float32)).astype(x.dtype)


def rotary(x, positions):
    half = HEAD_DIM // 2
    inv_freq = ROPE_BASE ** (-jnp.arange(half, dtype=jnp.float32) / half)
    ang = positions.astype(jnp.float32)[..., None] * inv_freq
    cos = jnp.cos(ang)[:, :, None, :]
    sin = jnp.sin(ang)[:, :, None, :]
    x1 = x[..., :half].astype(jnp.float32)
    x2 = x[..., half:].astype(jnp.float32)
    out = jnp.concatenate([x1 * cos - x2 * sin, x2 * cos + x1 * sin], axis=-1)
    return out.astype(x.dtype)


def retention(q, k, v, positions):
    B, S, H, D = q.shape
    C = RET_CHUNK
    NC = S // C
    q = rotary(q, positions)
    k = rotary(k, positions) * (D ** -0.5)
    log_g = jnp.log1p(-(2.0 ** (-5.0 - jnp.arange(H, dtype=jnp.float32))))
    idx = jnp.arange(C, dtype=jnp.float32)
    rel = idx[:, None] - idx[None, :]
    decay_intra = jnp.where(rel >= 0, jnp.exp(log_g[:, None, None] * jnp.maximum(rel, 0.0)), 0.0)
    xi = jnp.exp(log_g[:, None] * (idx + 1.0))
    zeta = jnp.exp(log_g[:, None] * (C - 1.0 - idx))
    chunk_decay = jnp.exp(log_g * C)
    qc = q.reshape(B, NC, C, H, D)
    kc = k.reshape(B, NC, C, H, D)
    vc = v.reshape(B, NC, C, H, D)
    scores = jnp.einsum('bnihd,bnjhd->bnhij', qc, kc) * decay_intra
    y_intra = jnp.einsum('bnhij,bnjhd->bnihd', scores, vc)
    kv = jnp.einsum('bnjhd,hj,bnjhe->nbhde', kc, zeta, vc).astype(jnp.float32)

    def step(state, kv_i):
        return chunk_decay[None, :, None, None] * state + kv_i, state

    _, states = lax.scan(step, jnp.zeros((B, H, D, D), jnp.float32), kv)
    y_cross = jnp.einsum('bnihd,nbhde,hi->bnihe', qc, states, xi)
    return (y_intra + y_cross).reshape(B, S, H, D)


def head_group_norm(y, gain):
    B, S, H, D = y.shape
    yf = y.astype(jnp.float32)
    mu = jnp.mean(yf, axis=-1, keepdims=True)
    var = jnp.mean(jnp.square(yf - mu), axis=-1, keepdims=True)
    yn = (yf - mu) * lax.rsqrt(var + EPS)
    return yn.reshape(B, S, H * D) * gain.astype(jnp.float32)


def stick_breaking(q, k, v):
    B, S, H, D = q.shape
    NB = S // SB_BLOCK
    q_blocks = q.reshape(B, NB, SB_BLOCK, H, D).transpose(1, 0, 3, 2, 4)
    kt = k.transpose(0, 2, 1, 3)
    vt = v.transpose(0, 2, 1, 3)
    key_pos = jnp.arange(S)
    scale = D ** -0.5

    def block(args):
        qi, start = args
        z = jnp.einsum('bhqd,bhsd->bhqs', qi, kt).astype(jnp.float32) * scale
        q_pos = start + jnp.arange(SB_BLOCK)
        mask = key_pos[None, :] < q_pos[:, None]
        log_beta = jax.nn.log_sigmoid(z)
        log_stay = jnp.where(mask, log_beta - z, 0.0)
        log_w = log_beta + lax.cumsum(log_stay, axis=3, reverse=True) - log_stay
        w = jnp.where(mask, jnp.exp(log_w), 0.0)
        return jnp.einsum('bhqs,bhsd->bhqd', w.astype(vt.dtype), vt)

    starts = jnp.arange(NB) * SB_BLOCK
    out = lax.map(block, (q_blocks, starts))
    return out.transpose(1, 0, 3, 2, 4).reshape(B, S, H, D)


def peer(h, w_query, sub_keys, expert_down, expert_up):
    B, S, Dm = h.shape
    T = B * S
    K = PEER_TOPK
    ht = h.reshape(T, Dm)
    qry = (ht @ w_query).reshape(T, PEER_HEADS, 2, PEER_HALF)
    sub_scores = jnp.einsum('thpc,hpkc->thpk', qry, sub_keys).astype(jnp.float32)
    top_s, top_i = lax.top_k(sub_scores, K)
    cand_s = top_s[:, :, 0, :, None] + top_s[:, :, 1, None, :]
    cand_id = top_i[:, :, 0, :, None] * N_KEYS + top_i[:, :, 1, None, :]
    best_s, best_c = lax.top_k(cand_s.reshape(T, PEER_HEADS, K * K), K)
    expert_id = jnp.take_along_axis(cand_id.reshape(T, PEER_HEADS, K * K), best_c, axis=-1)
    gate = jax.nn.softmax(best_s, axis=-1)
    NBK = T // PEER_BLOCK
    ids = expert_id.reshape(NBK, PEER_BLOCK, PEER_HEADS * K)
    gates = gate.reshape(NBK, PEER_BLOCK, PEER_HEADS * K)
    xs = ht.reshape(NBK, PEER_BLOCK, Dm)

    def block(args):
        xb, ib, gb = args
        u = expert_down[ib]
        a = jnp.einsum('pd,ped->pe', xb, u).astype(jnp.float32)
        act = jax.nn.gelu(a, approximate=False) * gb
        v = expert_up[ib]
        return jnp.einsum('pe,ped->pd', act.astype(xb.dtype), v)

    y = lax.map(block, (xs, ids, gates))
    return y.reshape(B, S, Dm)


def setup_inputs(seed: int = 0) -> dict:
    key = jax.random.key(seed)
    ks = jax.random.split(key, 16)
    f32 = jnp.float32
    nrm = lambda k, shape, s: jax.random.normal(k, shape, f32) * s
    return {
        "x": nrm(ks[0], (BATCH, SEQ, D_MODEL), 1.0),
        "c": nrm(ks[1], (BATCH, D_MODEL), 1.0),
        "positions": jnp.broadcast_to(jnp.arange(SEQ, dtype=jnp.int32)[None, :], (BATCH, SEQ)),
        "ada_w": nrm(ks[2], (DEPTH, D_MODEL, 6 * D_MODEL), D_MODEL ** -0.5),
        "ada_b": nrm(ks[3], (DEPTH, 6 * D_MODEL), 0.1),
        "norm1_gain": 1.0 + nrm(ks[4], (DEPTH, D_MODEL), 0.1),
        "norm2_gain": 1.0 + nrm(ks[5], (DEPTH, D_MODEL), 0.1),
        "w_in": nrm(ks[6], (DEPTH, D_MODEL, IN_COLS), D_MODEL ** -0.5),
        "ret_norm_gain": 1.0 + nrm(ks[7], (DEPTH, RET_WIDTH), 0.1),
        "sb_q_gain": 1.0 + nrm(ks[8], (DEPTH, HEAD_DIM), 0.1),
        "sb_k_gain": 1.0 + nrm(ks[9], (DEPTH, HEAD_DIM), 0.1),
        "sb_out_gain": 1.0 + nrm(ks[10], (DEPTH, HEAD_DIM), 0.1),
        "w_out": nrm(ks[11], (DEPTH, D_MIX, D_MODEL), D_MIX ** -0.5),
        "peer_w_query": nrm(ks[12], (DEPTH, D_MODEL, PEER_HEADS * PEER_QDIM), D_MODEL ** -0.5),
        "peer_sub_keys": nrm(ks[13], (DEPTH, PEER_HEADS, 2, N_KEYS, PEER_HALF), PEER_HALF ** -0.5),
        "peer_down": nrm(ks[14], (DEPTH, N_EXPERTS, D_MODEL), D_MODEL ** -0.5),
        "peer_up": nrm(ks[15], (DEPTH, N_EXPERTS, D_MODEL), PEER_HEADS ** -0.5),
    }


def reference(x, c, positions, ada_w, ada_b, norm1_gain, norm2_gain, w_in, ret_norm_gain,
              sb_q_gain, sb_k_gain, sb_out_gain, w_out, peer_w_query, peer_sub_keys,
              peer_down, peer_up):
    B, S, _ = x.shape
    for layer in range(DEPTH):
        mod = jax.nn.silu(c) @ ada_w[layer] + ada_b[layer]
        shift1, scale1, gate1, shift2, scale2, gate2 = jnp.split(mod[:, None, :], 6, axis=-1)

        h = rms_norm(x, norm1_gain[layer]) * (1.0 + scale1) + shift1
        proj = h @ w_in[layer]
        r_q, r_k, r_v, r_g, s_q, s_k, s_v = jnp.split(
            proj, np.cumsum([RET_WIDTH] * 4 + [SB_WIDTH] * 2), axis=-1)
        heads_r = lambda t: t.reshape(B, S, N_RET_HEADS, HEAD_DIM)
        heads_s = lambda t: t.reshape(B, S, N_SB_HEADS, HEAD_DIM)

        y_ret = retention(heads_r(r_q), heads_r(r_k), heads_r(r_v), positions)
        y_ret = head_group_norm(y_ret, ret_norm_gain[layer]) * jax.nn.silu(r_g.astype(jnp.float32))

        q_sb = rms_norm(heads_s(s_q), sb_q_gain[layer])
        k_sb = rms_norm(heads_s(s_k), sb_k_gain[layer])
        y_sb = stick_breaking(q_sb, k_sb, heads_s(s_v))
        y_sb = rms_norm(y_sb, sb_out_gain[layer]).reshape(B, S, SB_WIDTH)

        mixed = jnp.concatenate([y_ret.astype(x.dtype), y_sb.astype(x.dtype)], axis=-1) @ w_out[layer]
        x = x + gate1 * mixed

        h2 = rms_norm(x, norm2_gain[layer]) * (1.0 + scale2) + shift2
        ffn = peer(h2, peer_w_query[layer], peer_sub_keys[layer], peer_down[layer], peer_up[layer])
        x = x + gate2 * ffn
    return x
```

```python
from contextlib import ExitStack
import math
import numpy as np
import ml_dtypes

import concourse.bass as bass
import concourse.mybir as mybir
from concourse.bass_utils import run_bass_kernel_spmd

F32 = mybir.dt.float32
BF16 = mybir.dt.bfloat16
I32 = mybir.dt.int32
ALU = mybir.AluOpType
AF = mybir.ActivationFunctionType

D_MODEL = 1024
SEQ = 16384
NCORES = 8
EPS = 1e-6
N_KEYS = 128
PEER_HEADS = 8
TWO_PI = 2.0 * math.pi


class Prog:
    LIM = 30000
    SAME_ENGINE_SYNC = ("act", "dve", "pool")

    def __init__(self, nc):
        self.nc = nc
        self.names = ["pe", "act", "dve", "pool", "sp"]
        self.stream = {e: [] for e in self.names}
        self.n = {e: 0 for e in self.names}
        self.res = {}
        self.dma_cnt = {}
        self.dma_inc = {}
        self.all_dma_tokens = []

    def _deps(self, eng, r, w):
        deps = set()
        for k in r:
            st = self.res.get(k)
            if st and st[0] is not None:
                deps.add(st[0])
        for k in w:
            st = self.res.get(k)
            if st:
                if st[0] is not None:
                    deps.add(st[0])
                deps.update(st[1])
        out = []
        for t in deps:
            if t[0] == "e" and t[1] == eng and eng not in self.SAME_ENGINE_SYNC:
                continue
            out.append(t)
        return out

    def _commit(self, tok, r, w):
        for k in r:
            st = self.res.setdefault(k, [None, set()])
            st[1] = {t for t in st[1] if not (t[0] == tok[0] and t[1] == tok[1])}
            st[1].add(tok)
        for k in w:
            self.res[k] = [tok, set()]

    def op(self, eng, meth, r=(), w=(), *args, **kwargs):
        deps = self._deps(eng, r, w)
        idx = self.n[eng]
        self.n[eng] += 1
        self.stream[eng].append(("op", deps, meth, args, kwargs, idx))
        self._commit(("e", eng, idx), r, w)

    def dma(self, eng, key, out, in_, r=(), w=(), meth="dma_start", inc=16, **kwargs):
        deps = self._deps(eng, r, w)
        cnt = self.dma_cnt.get(key, 0) + 1
        self.dma_cnt[key] = cnt
        self.dma_inc[key] = inc
        kw = dict(kwargs)
        if out is not None:
            kw["out"] = out
        if in_ is not None:
            kw["in_"] = in_
        self.stream[eng].append(("dma", deps, key, meth, kw, inc))
        tok = ("d", key, cnt)
        self._commit(tok, r, w)
        self.all_dma_tokens.append(tok)
        return tok

    def wait_tokens(self, eng, toks):
        self.stream[eng].append(("wait", list(toks)))

    def barrier(self):
        last = {e: self.n[e] - 1 for e in self.names if self.n[e] > 0}
        dl = [("d", k, c) for k, c in self.dma_cnt.items()]
        for e in self.names:
            toks = [("e", o, i) for o, i in last.items() if o != e] + dl
            self.wait_tokens(e, toks)
        self.res = {}

    def emit(self, st):
        nc = self.nc
        sems = {}
        for e in self.names:
            nsem = max(1, (self.n[e] + self.LIM - 1) // self.LIM)
            sems[e] = [st.enter_context(nc.semaphore(f"pg_{e}_{i}")) for i in range(nsem)]
        dsem = {k: st.enter_context(nc.semaphore(f"dm_{k}")) for k in self.dma_cnt}
        LIM = self.LIM
        block = st.enter_context(nc.Block())

        def run(eng_name):
            def body(engine):
                waited = {}

                def do_wait(tok):
                    if tok[0] == "e":
                        s = sems[tok[1]][tok[2] // LIM]
                        v = tok[2] % LIM + 1
                        key = ("e", tok[1], tok[2] // LIM)
                        for kk, vv in waited.items():
                            if kk[0] == "e" and kk[1] == tok[1] and kk[2] > tok[2] // LIM:
                                return
                    else:
                        s = dsem[tok[1]]
                        v = (self.dma_inc[tok[1]] or 1) * tok[2]
                        key = ("d", tok[1])
                    if waited.get(key, 0) >= v:
                        return
                    waited[key] = v
                    engine.wait_ge(s, v)

                for item in self.stream[eng_name]:
                    if item[0] == "op":
                        _, deps, meth, args, kwargs, idx = item
                        for t in sorted(deps):
                            do_wait(t)
                        ins = getattr(engine, meth)(*args, **kwargs)
                        ins.then_inc(sems[eng_name][idx // LIM], 1)
                    elif item[0] == "dma":
                        _, deps, key, meth, kw, inc = item
                        for t in sorted(deps):
                            do_wait(t)
                        ins = getattr(engine, meth)(**kw)
                        if inc:
                            ins.then_inc(dsem[key], inc)
                        else:
                            ins.then_inc(dsem[key])
                    else:
                        for t in sorted(item[1]):
                            do_wait(t)
            return body

        block.tensor(run("pe"))
        block.scalar(run("act"))
        block.vector(run("dve"))
        block.gpsimd(run("pool"))
        block.sync(run("sp"))


class K:
    def __init__(self, nc, P, st, sb_bytes=206 * 1024):
        self.nc, self.P, self.st = nc, P, st
        self.big = st.enter_context(nc.sbuf_tensor("s_big", [128, sb_bytes // 4], F32))
        self.sb_bytes = sb_bytes
        self.sb_off = 0
        self.banks = [st.enter_context(nc.psum_tensor(f"p_bank{i}", [128, 512], F32)) for i in range(8)]
        self.bank_i = 0

    def reset(self):
        self.sb_off = 0
        self.bank_i = 0

    def sb(self, name, shape, dt):
        per = int(np.prod(shape[1:])) * mybir.dt.size(dt)
        per_al = (per + 31) // 32 * 32
        assert self.sb_off + per_al <= self.sb_bytes, (name, self.sb_off, per_al, self.sb_bytes)
        a = self.sb_off // 4
        v = self.big[0:shape[0], a:a + per_al // 4]
        if dt != F32:
            v = v.bitcast(dt)
        n = int(np.prod(shape[1:]))
        v = v[:, 0:n]
        if len(shape) == 3:
            v = v.rearrange("p (a b) -> p a b", a=shape[1])
        self.sb_off += per_al
        return v

    def ps(self, name, shape, dt):
        b = self.banks[self.bank_i]
        self.bank_i += 1
        v = b[:]
        if dt != F32:
            v = v.bitcast(dt)
        return v

    def rstd(self, out, ms, r, w, tmpkey, tmp, negh):
        P = self.P
        P.op("dve", "tensor_scalar", r, [tmpkey], out=tmp, in0=ms, scalar1=EPS, scalar2=None, op0=ALU.add)
        P.op("pool", "tensor_tensor", [tmpkey], w, out=out, in0=tmp, in1=negh, op=ALU.pow)


class Arena:
    def __init__(self, k, name, nbytes):
        self.t = k.sb(name, [128, nbytes // 4], F32)
        self.nbytes = nbytes
        self.off = 0

    def reset(self):
        self.off = 0

    def alloc(self, shape, dt):
        per = int(np.prod(shape[1:])) * mybir.dt.size(dt)
        per_al = (per + 31) // 32 * 32
        assert self.off + per_al <= self.nbytes, (self.off, per_al, self.nbytes)
        a = self.off // 4
        v = self.t[0:shape[0], a:a + per_al // 4]
        if dt != F32:
            v = v.bitcast(dt)
        n = int(np.prod(shape[1:]))
        v = v[:, 0:n]
        if len(shape) == 3:
            v = v.rearrange("p (a b) -> p a b", a=shape[1])
        self.off += per_al
        return v


def build_consts(head):
    C = 128
    g = 1.0 - 2.0 ** (-5.0 - head)
    lg = math.log1p(-(2.0 ** (-5.0 - head)))
    ir = np.arange(C)
    rj = ir[:, None].astype(np.float64)
    ri = ir[None, :].astype(np.float64)
    DT = np.where(rj >= ri, np.exp(lg * np.maximum(rj - ri, 0.0)), 0.0) * (64.0 ** -0.5)
    ctime = (C - 1 - ir).astype(np.float64)
    xi = np.exp(lg * (ctime + 1.0))
    zeta = np.exp(lg * (C - 1.0 - ctime)) * (64.0 ** -0.5)
    cd = math.exp(lg * C)
    xi_b = np.broadcast_to(xi[None, :], (64, C))
    half = 32
    inv_freq = (10000.0 ** (-np.arange(half, dtype=np.float32) / half)).astype(np.float32)
    sbmask = (ir[None, :] <= ir[:, None]).astype(np.float32)
    return dict(
        DT=np.ascontiguousarray(DT, dtype=np.float32),
        xi_b=np.ascontiguousarray(xi_b, dtype=np.float32),
        zeta8=np.ascontiguousarray(zeta[:, None], dtype=np.float32),
        cd=np.full((64, 1), cd, dtype=np.float32),
        invf=np.ascontiguousarray(np.broadcast_to(inv_freq[None, :], (128, half)), dtype=np.float32),
        sbmask=sbmask,
        ident=np.eye(128, dtype=np.float32),
    )


def emit_adaln(k, io, ar, mod_sb, modkey, col0, ncols, tag, pm, pmkey):
    nc, P = k.nc, k.P
    cT = ar.alloc([128, 8], F32)
    sg = ar.alloc([128, 8], F32)
    sc = ar.alloc([128, 8], F32)
    scb = ar.alloc([128, 8, 128], F32)
    P.dma("sp", f"cT{tag}", cT[:], io["cT"], w=[f"cT{tag}"])
    P.op("act", "activation", [f"cT{tag}"], [f"csg{tag}"], out=sg[:], in_=cT[:], func=AF.Sigmoid)
    P.op("dve", "tensor_tensor", [f"cT{tag}", f"csg{tag}"], [f"csc{tag}"], out=sc[:], in0=cT[:], in1=sg[:], op=ALU.mult)
    P.op("dve", "tensor_copy", [f"csc{tag}"], [f"cscb{tag}"], out=scb[:], in_=sc[:].unsqueeze(2).to_broadcast([128, 8, 128]))
    adaw = io["ada_w"].rearrange("(k p) n -> p k n", p=128)
    CW = 256
    wbuf = [ar.alloc([128, 8, CW], F32) for i in range(2)]
    bb = ar.alloc([128, ncols], F32)
    P.dma("sp", f"adab{tag}", bb[:], io["ada_b"][0:1, col0:col0 + ncols].partition_broadcast(128), w=[f"adab{tag}"])
    for g in range(ncols // CW):
        s = g % 2
        P.dma("sp", f"adaw{tag}{s}", wbuf[s][:], adaw[:, :, col0 + g * CW: col0 + (g + 1) * CW], w=[f"adaw{tag}{s}"])
        for kk in range(8):
            P.op("pe", "matmul", [f"cscb{tag}", f"adaw{tag}{s}"], [pmkey], out=pm[:, 0:CW], lhsT=scb[:, kk, :],
                 rhs=wbuf[s][:, kk, :], start=(kk == 0), stop=(kk == 7))
        P.op("dve", "tensor_tensor", [pmkey, f"adab{tag}"], [modkey], out=mod_sb[:, g * CW:(g + 1) * CW],
             in0=pm[:, 0:CW], in1=bb[:, g * CW:(g + 1) * CW], op=ALU.add)


def emit_phase_a(k, io, S, yout):
    nc, P = k.nc, k.P
    NT = S // 128
    sb, ps = k.sb, k.ps
    ar = Arena(k, "arenaA", 72 * 1024)

    hT_ps = [ps(f"hTps{i}", [128, 1024], BF16) for i in range(2)]
    pj_ps = [ps(f"pjps{i}", [128, 512], F32) for i in range(2)]
    trA = ps("trA", [128, 1024], BF16)
    trB = ps("trB", [128, 1024], BF16)
    rtA = ps("rtA", [128, 512], F32)
    rtB = ps("rtB", [128, 512], F32)

    ident_b = sb("ident_b", [128, 128], BF16)
    negh = sb("negh", [128, 1], F32)
    ones_f = sb("ones_f", [128, 512], F32)
    A1 = sb("A1", [128, 1024], F32)
    B1 = sb("B1", [128, 1024], F32)
    wc_b = sb("wc_b", [128, 8, 448], BF16)
    gb = sb("gb", [128, 256], F32)
    gq8 = sb("gq8", [128, 64], F32)
    DT = sb("DT", [128, 128], F32)
    xi_b = sb("xi_b", [64, 128], F32)
    zeta8 = sb("zeta8", [128, 1], F32)
    cd = sb("cd", [64, 1], F32)
    invf = sb("invf", [128, 32], F32)
    sbmask = sb("sbmask", [128, 128], F32)
    sinT = sb("sinT", [128, NT, 32], F32)
    cosT = sb("cosT", [128, NT, 32], F32)
    QT = sb("QT", [64, S], BF16)
    KT = sb("KT", [64, S], BF16)
    VA = sb("VA", [128, NT, 64], BF16)
    Sf = sb("Sf", [64, 64], F32)
    Sb_ = sb("Sb", [64, 64], BF16)

    ident_f = ar.alloc([128, 128], F32)
    P.dma("sp", "ident", ident_f[:], io["ident"], w=["ident_f"])
    P.op("dve", "tensor_copy", ["ident_f"], ["ident_b"], out=ident_b[:], in_=ident_f[:])
    P.op("dve", "memset", [], ["negh"], negh[:], -0.5)
    P.op("dve", "memset", [], ["ones_f"], ones_f[:], 1.0)
    P.op("dve", "memset", [], ["Sf"], Sf[:], 0.0)
    P.op("dve", "memset", [], ["Sb"], Sb_[:], 0.0)

    mod = ar.alloc([128, 2048], F32)
    emit_adaln(k, io, ar, mod, "modA", 0, 2048, "A", pj_ps[0], "pjps0")
    g1b = ar.alloc([128, 1024], F32)
    P.dma("sp", "g1b", g1b[:], io["g1"][0:1, :].partition_broadcast(128), w=["g1b"])
    P.op("dve", "scalar_tensor_tensor", ["modA", "g1b"], ["A1"], out=A1[:], in0=mod[:, 1024:2048], scalar=1.0,
         in1=g1b[:], op0=ALU.add, op1=ALU.mult)
    P.op("dve", "tensor_copy", ["modA"], ["B1"], out=B1[:], in_=mod[:, 0:1024])

    wc_f = ar.alloc([128, 4, 448], F32)
    wsrc = io["w_c"].rearrange("(k p) n -> p k n", p=128)
    for hh in range(2):
        P.dma("sp", "wc", wc_f[:], wsrc[:, hh * 4:(hh + 1) * 4, :], w=["wc_f"])
        P.op("act", "copy", ["wc_f"], ["wc_b"], out=wc_b[:, hh * 4:(hh + 1) * 4, :], in_=wc_f[:])

    P.dma("sp", "gb", gb[:], io["gains4"][0:1, :].partition_broadcast(128), w=["gb"])
    P.op("dve", "tensor_scalar", ["gb"], ["gq8"], out=gq8[:], in0=gb[:, 64:128], scalar1=0.125, scalar2=None, op0=ALU.mult)
    for nm, t in (("DT", DT), ("xi_b", xi_b), ("zeta8", zeta8), ("cd", cd), ("invf", invf), ("sbmask", sbmask)):
        P.dma("sp", nm, t[:], io[nm], w=[nm])

    posi = ar.alloc([128, NT], I32)
    posf = ar.alloc([128, NT], F32)
    P.dma("sp", "posi", posi[:], io["posT"], w=["posi"])
    P.op("dve", "tensor_copy", ["posi"], ["posf"], out=posf[:], in_=posi[:])
    CH = min(8, NT)
    ang = ar.alloc([128, CH, 32], F32)
    t_u = ar.alloc([128, CH, 32], F32)
    t_ki = ar.alloc([128, CH, 32], I32)
    t_kf = ar.alloc([128, CH, 32], F32)
    t_r = ar.alloc([128, CH, 32], F32)
    t_m = ar.alloc([128, CH, 32], F32)
    for c0 in range(0, NT, CH):
        P.op("dve", "tensor_tensor", ["posf", "invf"], ["ang"], out=ang[:],
             in0=posf[:, c0:c0 + CH].unsqueeze(2).to_broadcast([128, CH, 32]),
             in1=invf[:].unsqueeze(1).to_broadcast([128, CH, 32]), op=ALU.mult)
        for which, dst, shift in (("s", sinT, 0.0), ("c", cosT, math.pi / 2)):
            P.op("dve", "tensor_scalar", ["ang"], ["t_u"], out=t_u[:], in0=ang[:], scalar1=shift, scalar2=1.0 / TWO_PI,
                 op0=ALU.add, op1=ALU.mult)
            P.op("dve", "tensor_copy", ["t_u"], ["t_ki"], out=t_ki[:], in_=t_u[:])
            P.op("dve", "tensor_copy", ["t_ki"], ["t_kf"], out=t_kf[:], in_=t_ki[:])
            P.op("dve", "tensor_scalar", ["ang"], ["t_u"], out=t_u[:], in0=ang[:], scalar1=shift, scalar2=None, op0=ALU.add)
            P.op("dve", "scalar_tensor_tensor", ["t_kf", "t_u"], ["t_r"], out=t_r[:], in0=t_kf[:], scalar=-TWO_PI, in1=t_u[:],
                 op0=ALU.mult, op1=ALU.add)
            P.op("dve", "tensor_single_scalar", ["t_r"], ["t_m"], out=t_m[:], in_=t_r[:], scalar=math.pi, op=ALU.is_gt)
            P.op("dve", "scalar_tensor_tensor", ["t_m", "t_r"], ["t_u"], out=t_u[:], in0=t_m[:], scalar=-TWO_PI, in1=t_r[:],
                 op0=ALU.mult, op1=ALU.add)
            P.op("dve", "tensor_single_scalar", ["t_u"], ["t_m"], out=t_m[:], in_=t_u[:], scalar=-math.pi, op=ALU.is_lt)
            P.op("dve", "scalar_tensor_tensor", ["t_m", "t_u"], ["t_r"], out=t_r[:], in0=t_m[:], scalar=TWO_PI, in1=t_u[:],
                 op0=ALU.mult, op1=ALU.add)
            P.op("dve", "tensor_scalar", ["t_r"], ["t_u"], out=t_u[:], in0=t_r[:], scalar1=3.1415925, scalar2=-3.1415925,
                 op0=ALU.min, op1=ALU.max)
            P.op("act", "activation", ["t_u"], [f"trig{which}"], out=dst[:, c0:c0 + CH, :], in_=t_u[:], func=AF.Sin)

    P.barrier()
    ar.reset()

    xt = [ar.alloc([128, 1024], F32) for i in range(2)]
    xs = [ar.alloc([128, 1024], F32) for i in range(2)]
    hb = [ar.alloc([128, 1024], BF16) for i in range(2)]
    hT = [ar.alloc([128, 1024], BF16) for i in range(2)]
    pj = [ar.alloc([128, 448], F32) for i in range(2)]
    yrs = [ar.alloc([128, 64], BF16) for i in range(2)]
    st6 = ar.alloc([128, 2, 6], F32)
    sm = ar.alloc([128, 64], F32)
    mv, ms, tmp1, rs = sm[:, 0:2], sm[:, 2:3], sm[:, 3:4], sm[:, 4:5]
    mvr, tmp2, rsr = sm[:, 5:7], sm[:, 7:8], sm[:, 8:9]
    mvq, msq, tmp3, rsq = sm[:, 9:11], sm[:, 11:12], sm[:, 12:13], sm[:, 13:14]
    mvk, msk, tmp4, rsk = sm[:, 14:16], sm[:, 16:17], sm[:, 17:18], sm[:, 18:19]
    st6r = sm[:, 20:26]
    st6q = sm[:, 26:32]
    st6k = sm[:, 32:38]
    tA = ar.alloc([128, 128], F32)
    tB1 = ar.alloc([128, 2, 32], F32)
    tB2 = ar.alloc([128, 2, 32], F32)
    rot = ar.alloc([128, 128], BF16)
    kz = ar.alloc([128, 64], BF16)
    vb = ar.alloc([128, 64], BF16)
    qT = ar.alloc([64, 128], BF16)
    kT = ar.alloc([64, 128], BF16)
    qxT = ar.alloc([64, 128], BF16)
    PT = ar.alloc([128, 128], BF16)
    ysb = ar.alloc([128, 64], F32)
    yn = ar.alloc([128, 64], F32)
    sgt = ar.alloc([128, 64], F32)
    gg = ar.alloc([128, 64], F32)
    qn = ar.alloc([128, 64], BF16)
    kn = ar.alloc([128, 64], BF16)

    xr = io["xr"]

    def load_x(it):
        n = NT - 1 - it
        s = it % 2
        P.dma("sp", f"xt{s}", xt[s][:], xr[n * 128:(n + 1) * 128, :], w=[f"xt{s}"])

    load_x(0)

    def P1(it):
        n = NT - 1 - it
        s = it % 2
        X, XS, HB, HT, PJ = f"xt{s}", f"xs{s}", f"hb{s}", f"hT{s}", f"pj{s}"
        if it + 1 < NT:
            load_x(it + 1)
        P.op("dve", "bn_stats", [X], ["st6"], out=st6[:, 0, :], in_=xt[s][:, 0:512])
        P.op("dve", "bn_stats", [X], ["st6"], out=st6[:, 1, :], in_=xt[s][:, 512:1024])
        P.op("dve", "bn_aggr", ["st6"], ["mv"], out=mv, in_=st6[:].rearrange("p a b -> p (a b)"))
        P.op("dve", "scalar_tensor_tensor", ["mv"], ["ms"], out=ms, in0=mv[:, 0:1], scalar=mv[:, 0:1], in1=mv[:, 1:2],
             op0=ALU.mult, op1=ALU.add)
        k.rstd(rs, ms, ["ms"], ["rs"], "tmp1", tmp1, negh[:])
        P.op("dve", "scalar_tensor_tensor", [X, "rs", "A1"], [XS], out=xs[s][:], in0=xt[s][:], scalar=rs[:, 0:1], in1=A1[:],
             op0=ALU.mult, op1=ALU.mult)
        P.op("pool", "tensor_tensor", [XS, "B1"], [HB], out=hb[s][:], in0=xs[s][:], in1=B1[:], op=ALU.add)

    def P2(it):
        n = NT - 1 - it
        s = it % 2
        X, XS, HB, HT, PJ = f"xt{s}", f"xs{s}", f"hb{s}", f"hT{s}", f"pj{s}"
        for kk in range(8):
            P.op("pe", "transpose", [HB, "ident_b"], [f"hTps{s}"], out=hT_ps[s][:, kk * 128:(kk + 1) * 128],
                 in_=hb[s][:, kk * 128:(kk + 1) * 128], identity=ident_b[:])
        P.op("act", "copy", [f"hTps{s}"], [HT], out=hT[s][:], in_=hT_ps[s][:])
        for kk in range(8):
            P.op("pe", "matmul", [HT, "wc_b"], [f"pjps{s}"], out=pj_ps[s][:, 0:448], lhsT=hT[s][:, kk * 128:(kk + 1) * 128],
                 rhs=wc_b[:, kk, :], start=(kk == 0), stop=(kk == 7))
        P.op("act", "copy", [f"pjps{s}"], [PJ], out=pj[s][:], in_=pj_ps[s][:, 0:448])


    def P3(it):
        n = NT - 1 - it
        s = it % 2
        X, XS, HB, HT, PJ = f"xt{s}", f"xs{s}", f"hb{s}", f"hT{s}", f"pj{s}"
        pjt = pj[s]
        qk4 = pjt[:, 0:128].rearrange("p (a h f) -> p a h f", a=2, h=2)
        cos_n = cosT[:, n, :]
        sin_n = sinT[:, n, :]
        tA4 = tA[:].rearrange("p (a h f) -> p a h f", a=2, h=2)
        rot4 = rot[:].rearrange("p (a h f) -> p a h f", a=2, h=2)
        P.op("dve", "tensor_tensor", [PJ, "trigc"], ["tA"], out=tA4, in0=qk4,
             in1=cos_n.unsqueeze(1).unsqueeze(1).to_broadcast([128, 2, 2, 32]), op=ALU.mult)
        P.op("dve", "tensor_tensor", [PJ, "trigs"], ["tB1"], out=tB1[:], in0=qk4[:, :, 1, :],
             in1=sin_n.unsqueeze(1).to_broadcast([128, 2, 32]), op=ALU.mult)
        P.op("dve", "tensor_tensor", [PJ, "trigs"], ["tB2"], out=tB2[:], in0=qk4[:, :, 0, :],
             in1=sin_n.unsqueeze(1).to_broadcast([128, 2, 32]), op=ALU.mult)
        P.op("dve", "tensor_tensor", ["tA", "tB1"], ["rot"], out=rot4[:, :, 0, :], in0=tA4[:, :, 0, :], in1=tB1[:], op=ALU.subtract)
        P.op("dve", "tensor_tensor", ["tA", "tB2"], ["rot"], out=rot4[:, :, 1, :], in0=tA4[:, :, 1, :], in1=tB2[:], op=ALU.add)
        P.op("dve", "tensor_scalar", ["rot", "zeta8"], ["kz"], out=kz[:], in0=rot[:, 64:128], scalar1=zeta8[:, 0:1], scalar2=None,
             op0=ALU.mult)
        P.op("act", "copy", [PJ], ["vb"], out=vb[:], in_=pjt[:, 128:192])
        P.op("pe", "transpose", ["rot", "ident_b"], ["trA"], out=trA[0:64, 0:128], in_=rot[:, 0:64], identity=ident_b[:])
        P.op("pe", "transpose", ["rot", "ident_b"], ["trA"], out=trA[0:64, 128:256], in_=rot[:, 64:128], identity=ident_b[:])
        P.op("act", "copy", ["trA"], ["qT"], out=qT[:], in_=trA[0:64, 0:128])
        P.op("act", "copy", ["trA"], ["kT"], out=kT[:], in_=trA[0:64, 128:256])
        P.op("dve", "tensor_tensor", ["qT", "xi_b"], ["qxT"], out=qxT[:], in0=qT[:], in1=xi_b[:], op=ALU.mult)
        P.op("pe", "matmul", ["kT", "qT"], ["rtA"], out=rtA[:, 0:128], lhsT=kT[:], rhs=qT[:], start=True, stop=True)
        P.op("dve", "tensor_tensor", ["rtA", "DT"], ["PT"], out=PT[:], in0=rtA[:, 0:128], in1=DT[:], op=ALU.mult)
        P.op("pe", "matmul", ["PT", "vb"], ["rtB"], out=rtB[:, 0:64], lhsT=PT[:], rhs=vb[:], start=True, stop=False)
        P.op("pe", "matmul", ["qxT", "Sb"], ["rtB"], out=rtB[:, 0:64], lhsT=qxT[:], rhs=Sb_[:], start=False, stop=True)
        P.op("pe", "matmul", ["kz", "vb"], ["rtA"], out=rtA[0:64, 128:192], lhsT=kz[:], rhs=vb[:], start=True, stop=True)
        P.op("dve", "scalar_tensor_tensor", ["Sf", "cd", "rtA"], ["Sf"], out=Sf[:], in0=Sf[:], scalar=cd[:, 0:1],
             in1=rtA[0:64, 128:192], op0=ALU.mult, op1=ALU.add)
        P.op("dve", "tensor_copy", ["Sf"], ["Sb"], out=Sb_[:], in_=Sf[:])
        P.op("act", "copy", ["rtB"], ["ysb"], out=ysb[:], in_=rtB[:, 0:64])
        P.op("dve", "bn_stats", ["ysb"], ["st6r"], out=st6r, in_=ysb[:])
        P.op("dve", "bn_aggr", ["st6r"], ["mvr"], out=mvr, in_=st6r)
        k.rstd(rsr, mvr[:, 1:2], ["mvr"], ["rsr"], "tmp2", tmp2, negh[:])
        P.op("dve", "tensor_scalar", ["ysb", "mvr", "rsr"], ["yn"], out=yn[:], in0=ysb[:], scalar1=mvr[:, 0:1], scalar2=rsr[:, 0:1],
             op0=ALU.subtract, op1=ALU.mult)
        P.op("act", "activation", [PJ], ["sgt"], out=sgt[:], in_=pjt[:, 192:256], func=AF.Sigmoid)
        P.op("dve", "tensor_tensor", [PJ, "sgt"], ["gg"], out=gg[:], in0=pjt[:, 192:256], in1=sgt[:], op=ALU.mult)
        P.op("dve", "tensor_tensor", ["gg", "gb"], ["gg"], out=gg[:], in0=gg[:], in1=gb[:, 0:64], op=ALU.mult)
        P.op("dve", "tensor_tensor", ["yn", "gg"], [f"yrs{s}"], out=yrs[s][:], in0=yn[:], in1=gg[:], op=ALU.mult)
        P.dma("pool", f"yr{s}", yout[n * 128:(n + 1) * 128, 0:64], yrs[s][:], r=[f"yrs{s}"], w=[])

        P.op("dve", "bn_stats", [PJ], ["st6q"], out=st6q, in_=pjt[:, 256:320])
        P.op("dve", "bn_aggr", ["st6q"], ["mvq"], out=mvq, in_=st6q)
        P.op("dve", "scalar_tensor_tensor", ["mvq"], ["msq"], out=msq, in0=mvq[:, 0:1], scalar=mvq[:, 0:1], in1=mvq[:, 1:2],
             op0=ALU.mult, op1=ALU.add)
        k.rstd(rsq, msq, ["msq"], ["rsq"], "tmp3", tmp3, negh[:])
        P.op("dve", "scalar_tensor_tensor", [PJ, "rsq", "gq8"], ["qn"], out=qn[:], in0=pjt[:, 256:320], scalar=rsq[:, 0:1],
             in1=gq8[:], op0=ALU.mult, op1=ALU.mult)
        P.op("dve", "bn_stats", [PJ], ["st6k"], out=st6k, in_=pjt[:, 320:384])
        P.op("dve", "bn_aggr", ["st6k"], ["mvk"], out=mvk, in_=st6k)
        P.op("dve", "scalar_tensor_tensor", ["mvk"], ["msk"], out=msk, in0=mvk[:, 0:1], scalar=mvk[:, 0:1], in1=mvk[:, 1:2],
             op0=ALU.mult, op1=ALU.add)
        k.rstd(rsk, msk, ["msk"], ["rsk"], "tmp4", tmp4, negh[:])
        P.op("dve", "scalar_tensor_tensor", [PJ, "rsk", "gb"], ["kn"], out=kn[:], in0=pjt[:, 320:384], scalar=rsk[:, 0:1],
             in1=gb[:, 128:192], op0=ALU.mult, op1=ALU.mult)
        P.op("pe", "transpose", ["qn", "ident_b"], ["trB"], out=trB[0:64, 0:128], in_=qn[:], identity=ident_b[:])
        P.op("pe", "transpose", ["kn", "ident_b"], ["trB"], out=trB[0:64, 128:256], in_=kn[:], identity=ident_b[:])
        P.op("act", "copy", ["trB"], ["QT"], out=QT[:, n * 128:(n + 1) * 128], in_=trB[0:64, 0:128])
        P.op("act", "copy", ["trB"], ["KT"], out=KT[:, n * 128:(n + 1) * 128], in_=trB[0:64, 128:256])
        P.op("act", "copy", [PJ], ["VA"], out=VA[:, n, :], in_=pjt[:, 384:448])


    for step in range(NT + 2):
        if step < NT:
            P1(step)
        if 0 <= step - 1 < NT:
            P2(step - 1)
        if step - 2 >= 0:
            P3(step - 2)
    P.barrier()
    ar.reset()

    NZ = 3
    om = [ar.alloc([128, 512], F32) for i in range(NZ)]
    Qb = [ar.alloc([128, 520], F32) for i in range(NZ)]
    wq = [ar.alloc([128, 512], BF16) for i in range(4)]
    wT = [ar.alloc([128, 512], BF16) for i in range(2)]
    ysb_s = [ar.alloc([128, 64], BF16) for i in range(2)]
    asb = ar.alloc([128, 64], F32)
    sm2 = ar.alloc([128, 16], F32)
    st6o, mvo, mso, tmp5, rso = sm2[:, 0:6], sm2[:, 6:8], sm2[:, 8:9], sm2[:, 9:10], sm2[:, 10:11]
    z_ps = [pj_ps[0], pj_ps[1], trA.bitcast(F32)]
    z_key = ["pjps0", "pjps1", "trA"]
    wT_ps = hT_ps
    jobs = []
    for qb in range(NT):
        tl = [(kb0, min(4, NT - kb0)) for kb0 in range(qb, NT, 4)]
        for tix, (kb0, nb) in enumerate(tl):
            jobs.append((qb, tix, kb0, nb, tix == len(tl) - 1))

    def S1(i):
        qb, tix, kb0, nb, lastt = jobs[i]
        W = 128 * nb
        s = i % NZ
        ZP, OM, QB, WQ = z_key[s], f"om{s}", f"Qb{s}", f"wq{i % 4}"
        QC = f"Qc{s}"
        P.op("pe", "matmul", ["QT", "KT"], [ZP], out=z_ps[s][:, 0:W], lhsT=QT[:, qb * 128:(qb + 1) * 128],
             rhs=KT[:, kb0 * 128: kb0 * 128 + W], start=True, stop=True)
        P.op("act", "activation", [ZP], [OM], out=om[s][:, 0:W], in_=z_ps[s][:, 0:W], func=AF.Sigmoid, scale=-1.0)
        if tix == 0:
            P.op("dve", "tensor_tensor", [OM, "sbmask"], [OM], out=om[s][:, 0:128], in0=om[s][:, 0:128], in1=sbmask[:], op=ALU.max)
            P.op("dve", "memset", [], [QC], Qb[s][:, 0:1], 1.0)
            init_ap, init_key = Qb[s][:, 0:1], QC
        else:
            ps_ = (i - 1) % NZ
            pW = 128 * jobs[i - 1][3]
            P.op("act", "copy", [f"Qb{ps_}"], [QC], out=Qb[s][:, 0:1], in_=Qb[ps_][:, pW:pW + 1])
            init_ap, init_key = Qb[ps_][:, pW:pW + 1], f"Qb{ps_}"
        P.op("dve", "tensor_tensor_scan", [OM, init_key, "ones_f"], [QB], out=Qb[s][:, 1:W + 1], data0=om[s][:, 0:W],
             data1=ones_f[:, 0:W], initial=init_ap, op0=ALU.mult, op1=ALU.mult)
        P.op("pool", "tensor_tensor", [QB, QC], [WQ], out=wq[i % 4][:, 0:W], in0=Qb[s][:, 0:W], in1=Qb[s][:, 1:W + 1], op=ALU.subtract)

    def S2(i):
        qb, tix, kb0, nb, lastt = jobs[i]
        W = 128 * nb
        s = i % 2
        a_s = qb % 2
        ACC = "rtA" if a_s == 0 else "rtB"
        acc = (rtA if a_s == 0 else rtB)[:, 256:320]
        WQ, WTP, WT = f"wq{i % 4}", f"hTps{s}", f"wT{s}"
        for j in range(nb):
            P.op("pe", "transpose", [WQ, "ident_b"], [WTP], out=wT_ps[s][:, j * 128:(j + 1) * 128],
                 in_=wq[i % 4][:, j * 128:(j + 1) * 128], identity=ident_b[:])
        P.op("act", "copy", [WTP], [WT], out=wT[s][:, 0:W], in_=wT_ps[s][:, 0:W])
        for j in range(nb):
            P.op("pe", "matmul", [WT, "VA"], [ACC], out=acc, lhsT=wT[s][:, j * 128:(j + 1) * 128], rhs=VA[:, kb0 + j, :],
                 start=(tix == 0 and j == 0), stop=(lastt and j == nb - 1))
        if lastt:
            P.op("act", "copy", [ACC], ["asb"], out=asb[:], in_=acc)
            P.op("dve", "bn_stats", ["asb"], ["st6o"], out=st6o, in_=asb[:])
            P.op("dve", "bn_aggr", ["st6o"], ["mvo"], out=mvo, in_=st6o)
            P.op("dve", "scalar_tensor_tensor", ["mvo"], ["mso"], out=mso, in0=mvo[:, 0:1], scalar=mvo[:, 0:1], in1=mvo[:, 1:2],
                 op0=ALU.mult, op1=ALU.add)
            k.rstd(rso, mso, ["mso"], ["rso"], "tmp5", tmp5, negh[:])
            P.op("dve", "scalar_tensor_tensor", ["asb", "rso", "gb"], [f"ysbs{a_s}"], out=ysb_s[a_s][:], in0=asb[:],
                 scalar=rso[:, 0:1], in1=gb[:, 192:256], op0=ALU.mult, op1=ALU.mult)
            P.dma("pool", f"ys{a_s}", yout[qb * 128:(qb + 1) * 128, 64:128], ysb_s[a_s][:], r=[f"ysbs{a_s}"], w=[])

    DEPTH = 2
    for i in range(len(jobs) + DEPTH):
        if i < len(jobs):
            S1(i)
        if i - DEPTH >= 0:
            S2(i - DEPTH)
    P.barrier()


def emit_phase_b(k, io, T, load_y, out):
    nc, P = k.nc, k.P
    NTB = T // 128
    sb, ps = k.sb, k.ps
    ar = Arena(k, "arenaB", 144 * 1024)
    NG = T // 256
    x1d = nc.dram_tensor("x1d", [T, 1024], F32, kind="Internal").ap()
    GTd = nc.dram_tensor("GTd", [128, 128, T], BF16, kind="Internal").ap()

    trb = ps("b_tr", [128, 1024], BF16)
    mm = [ps(f"b_mm{i}", [128, 512], F32) for i in range(2)]
    gtp = [ps(f"b_gt{i}", [128, 512], F32) for i in range(4)]
    xtra = ps("b_x", [128, 512], F32)

    identb = sb("b_identb", [128, 128], BF16)
    identf = sb("b_identf", [128, 128], F32)
    negh = sb("b_negh", [128, 1], F32)
    modB = sb("b_mod", [128, 4096], F32)
    A2 = sb("b_A2", [128, 1024], F32)
    h2T = sb("b_h2T", [128, 8, T], BF16)
    gate1, B2, gate2 = modB[:, 0:1024], modB[:, 1024:2048], modB[:, 3072:4096]

    P.dma("sp", "b_ident", identf[:], io["ident"], w=["identf"])
    P.op("dve", "tensor_copy", ["identf"], ["identb"], out=identb[:], in_=identf[:])
    P.op("dve", "memset", [], ["negh"], negh[:], -0.5)
    emit_adaln(k, io, ar, modB, "modB", 2048, 4096, "B", mm[0], "mm0")
    g2b = ar.alloc([128, 1024], F32)
    P.dma("sp", "b_g2b", g2b[:], io["g2"][0:1, :].partition_broadcast(128), w=["g2b"])
    P.op("dve", "scalar_tensor_tensor", ["modB", "g2b"], ["A2"], out=A2[:], in0=modB[:, 2048:3072], scalar=1.0,
         in1=g2b[:], op0=ALU.add, op1=ALU.mult)
    P.barrier()
    ar.reset()

    wo_b = ar.alloc([128, 8, 1024], BF16)
    wq_b = ar.alloc([128, 8, 2048], BF16)
    skT = ar.alloc([128, 16, 128], F32)
    xt = [ar.alloc([128, 1024], F32) for i in range(2)]
    wst = xt
    qsrc = io["w_query"].rearrange("(k p) n -> p k n", p=128)
    ci = 0
    for kk in range(8):
        s = ci % 2; ci += 1
        for u in range(2):
            P.dma("sp", f"b_xt{s}", wst[s][u * 64:(u + 1) * 64, :], io["w_out"][u * 512 + kk * 64: u * 512 + (kk + 1) * 64, :],
                  w=[f"b_xt{s}"])
        P.op("act", "copy", [f"b_xt{s}"], ["wo_b"], out=wo_b[:, kk, :], in_=wst[s][:])
        for hh in range(2):
            s = ci % 2; ci += 1
            P.dma("sp", f"b_xt{s}", wst[s][:], qsrc[:, kk, hh * 1024:(hh + 1) * 1024], w=[f"b_xt{s}"])
            P.op("dve", "tensor_copy", [f"b_xt{s}"], ["wq_b"], out=wq_b[:, kk, hh * 1024:(hh + 1) * 1024], in_=wst[s][:])
    for g in range(16):
        s = ci % 2; ci += 1
        P.dma("sp", f"b_xt{s}", wst[s][:, 0:128], io["sub_keys"][g], w=[f"b_xt{s}"])
        P.op("pe", "transpose", [f"b_xt{s}", "identf"], ["xtra"], out=xtra[:, 0:128], in_=wst[s][:, 0:128], identity=identf[:])
        P.op("act", "copy", ["xtra"], ["skT"], out=skT[:, g, :], in_=xtra[:, 0:128])

    yt = [ar.alloc([128, 1024], BF16) for i in range(2)]
    yT = ar.alloc([128, 1024], BF16)
    x1t = [ar.alloc([128, 1024], F32) for i in range(2)]
    tmpm = ar.alloc([128, 1024], F32)
    xs2 = tmpm
    h2b = ar.alloc([128, 1024], BF16)
    qryT = ar.alloc([128, 16, 128], F32)
    ssb = ar.alloc([128, 16, 128], F32)
    ssk = ar.alloc([128, 16, 128], F32)
    top = ar.alloc([128, 16, 16], F32)
    cand = ar.alloc([128, 8, 256], F32)
    candk = ssk.rearrange("p a b -> p (a b)").rearrange("p (a b) -> p a b", a=8)
    best = ar.alloc([128, 8, 16], F32)
    bm = ar.alloc([128, 8, 16], F32)
    be = ar.alloc([128, 8, 16], F32)
    sm = ar.alloc([128, 64], F32)
    st6 = sm[:, 0:12]
    mv, ms, tmp1, rs = sm[:, 12:14], sm[:, 14:15], sm[:, 15:16], sm[:, 16:17]
    Zs, lnZ, biasT = sm[:, 20:28], sm[:, 28:36], sm[:, 36:44]
    Sg = [ar.alloc([128, 8, 128], F32) for i in range(2)]
    Eg = [ar.alloc([128, 8, 128], F32) for i in range(2)]
    Tg = [ar.alloc([128, 8, 128], BF16) for i in range(2)]
    gts = [ar.alloc([128, 8, 128], BF16) for i in range(2)]

    xsrc = io["xs"]
    load_y(0, yt[0], "b_yt0")
    for j in range(NTB):
        s = j % 2
        if j + 1 < NTB:
            load_y(j + 1, yt[1 - s], f"b_yt{1 - s}")
        P.dma("sp", f"b_xt{s}", xt[s][:], xsrc[j * 128:(j + 1) * 128, :], w=[f"b_xt{s}"])
        for kk in range(8):
            P.op("pe", "transpose", [f"b_yt{s}", "identb"], ["trb"], out=trb[:, kk * 128:(kk + 1) * 128],
                 in_=yt[s][:, kk * 128:(kk + 1) * 128], identity=identb[:])
        P.op("act", "copy", ["trb"], ["yT"], out=yT[:], in_=trb[:])
        for dh in range(2):
            for kk in range(8):
                P.op("pe", "matmul", ["yT", "wo_b"], [f"mm{dh}"], out=mm[dh][:], lhsT=yT[:, kk * 128:(kk + 1) * 128],
                     rhs=wo_b[:, kk, dh * 512:(dh + 1) * 512], start=(kk == 0), stop=(kk == 7))
            P.op("dve", "tensor_tensor", [f"mm{dh}", "modB"], ["tmpm"], out=tmpm[:, dh * 512:(dh + 1) * 512], in0=mm[dh][:],
                 in1=gate1[:, dh * 512:(dh + 1) * 512], op=ALU.mult)
        P.op("pool", "tensor_tensor", ["tmpm", f"b_xt{s}"], [f"x1t{s}"], out=x1t[s][:], in0=tmpm[:], in1=xt[s][:], op=ALU.add)
        P.dma("pool", f"b_x1s{s}", x1d[j * 128:(j + 1) * 128, :], x1t[s][:], r=[f"x1t{s}"], w=["x1d"])
        P.op("dve", "bn_stats", [f"x1t{s}"], ["st6"], out=st6[:, 0:6], in_=x1t[s][:, 0:512])
        P.op("dve", "bn_stats", [f"x1t{s}"], ["st6"], out=st6[:, 6:12], in_=x1t[s][:, 512:1024])
        P.op("dve", "bn_aggr", ["st6"], ["mv"], out=mv, in_=st6)
        P.op("dve", "scalar_tensor_tensor", ["mv"], ["ms"], out=ms, in0=mv[:, 0:1], scalar=mv[:, 0:1], in1=mv[:, 1:2],
             op0=ALU.mult, op1=ALU.add)
        k.rstd(rs, ms, ["ms"], ["rs"], "tmp1", tmp1, negh[:])
        P.op("dve", "scalar_tensor_tensor", [f"x1t{s}", "rs", "A2"], ["tmpm"], out=xs2[:], in0=x1t[s][:], scalar=rs[:, 0:1],
             in1=A2[:], op0=ALU.mult, op1=ALU.mult)
        P.op("pool", "tensor_tensor", ["tmpm", "modB"], ["h2b"], out=h2b[:], in0=xs2[:], in1=B2, op=ALU.add)
        for kk in range(8):
            P.op("pe", "transpose", ["h2b", "identb"], ["trb"], out=trb[:, kk * 128:(kk + 1) * 128],
                 in_=h2b[:, kk * 128:(kk + 1) * 128], identity=identb[:])
        P.op("act", "copy", ["trb"], ["h2T"], out=h2T[:, :, j * 128:(j + 1) * 128],
             in_=trb[:].rearrange("p (a b) -> p a b", a=8))
        for g4 in range(4):
            mb = g4 % 2
            for gi in range(4):
                g = g4 * 4 + gi
                for kk in range(8):
                    P.op("pe", "matmul", ["wq_b", "h2T"], [f"mm{mb}"], out=mm[mb][:, gi * 128:(gi + 1) * 128],
                         lhsT=wq_b[:, kk, g * 128:(g + 1) * 128], rhs=h2T[:, kk, j * 128:(j + 1) * 128],
                         start=(kk == 0), stop=(kk == 7))
            P.op("act", "copy", [f"mm{mb}"], ["qryT"], out=qryT[:, g4 * 4:(g4 + 1) * 4, :],
                 in_=mm[mb][:].rearrange("p (a b) -> p a b", a=4))
        for g4 in range(4):
            mb = g4 % 2
            for gi in range(4):
                g = g4 * 4 + gi
                P.op("pe", "matmul", ["qryT", "skT"], [f"mm{mb}"], out=mm[mb][:, gi * 128:(gi + 1) * 128],
                     lhsT=qryT[:, g, :], rhs=skT[:, g, :], start=True, stop=True)
            P.op("act", "copy", [f"mm{mb}"], ["ssb"], out=ssb[:, g4 * 4:(g4 + 1) * 4, :],
                 in_=mm[mb][:].rearrange("p (a b) -> p a b", a=4))
        for g in range(16):
            P.op("dve", "max", ["ssb"], ["top"], out=top[:, g, 0:8], in_=ssb[:, g, :])
            P.op("dve", "match_replace", ["ssb", "top"], ["ssk"], out=ssk[:, g, :], in_to_replace=top[:, g, 0:8],
                 in_values=ssb[:, g, :], imm_value=-1e30)
            P.op("dve", "max", ["ssk"], ["top"], out=top[:, g, 8:16], in_=ssk[:, g, :])
        for h in range(8):
            P.op("dve", "tensor_tensor", ["top"], ["cand"], out=cand[:, h, :].rearrange("p (a b) -> p a b", a=16),
                 in0=top[:, 2 * h, :].unsqueeze(2).to_broadcast([128, 16, 16]),
                 in1=top[:, 2 * h + 1, :].unsqueeze(1).to_broadcast([128, 16, 16]), op=ALU.add)
            P.op("dve", "max", ["cand"], ["best"], out=best[:, h, 0:8], in_=cand[:, h, :])
            P.op("dve", "match_replace", ["cand", "best"], ["ssk"], out=candk[:, h, :], in_to_replace=best[:, h, 0:8],
                 in_values=cand[:, h, :], imm_value=-1e30)
            P.op("dve", "max", ["ssk"], ["best"], out=best[:, h, 8:16], in_=candk[:, h, :])
        P.op("dve", "tensor_tensor", ["best"], ["bm"], out=bm[:], in0=best[:], in1=best[:, :, 0:1].to_broadcast([128, 8, 16]),
             op=ALU.subtract)
        P.op("act", "activation", ["bm"], ["be"], out=be[:], in_=bm[:], func=AF.Exp)
        P.op("dve", "tensor_reduce", ["be"], ["Zs"], out=Zs, in_=be[:], axis=mybir.AxisListType.X, op=ALU.add)
        P.op("act", "activation", ["Zs"], ["lnZ"], out=lnZ, in_=Zs, func=AF.Ln)
        P.op("dve", "scalar_tensor_tensor", ["best", "lnZ"], ["biasT"], out=biasT, in0=best[:, :, 0], scalar=-1.0, in1=lnZ,
             op0=ALU.mult, op1=ALU.subtract)
        def G1(i, j=j):
            c16, h = i // 8, i % 8
            s2 = i % 2
            P.op("dve", "tensor_tensor", ["ssb"], [f"Sg{s2}"], out=Sg[s2][:],
                 in0=ssb[:, 2 * h, c16 * 8:(c16 + 1) * 8].unsqueeze(2).to_broadcast([128, 8, 128]),
                 in1=ssb[:, 2 * h + 1, :].unsqueeze(1).to_broadcast([128, 8, 128]), op=ALU.add)
            P.op("act", "activation", [f"Sg{s2}", "biasT"], [f"Eg{s2}"], out=Eg[s2][:], in_=Sg[s2][:], func=AF.Exp,
                 bias=biasT[:, h:h + 1])

        def G2(i, j=j):
            c16, h = i // 8, i % 8
            s2 = i % 2
            gp = (c16 % 2) * 2
            GP = [f"gtp{gp}", f"gtp{gp + 1}"]
            P.op("dve", "scalar_tensor_tensor", [f"Sg{s2}", "best", f"Eg{s2}"], [f"Tg{s2}"], out=Tg[s2][:], in0=Sg[s2][:],
                 scalar=best[:, h, 15:16], in1=Eg[s2][:], op0=ALU.is_ge, op1=ALU.mult)
            for j1 in range(8):
                bank = gtp[gp + j1 // 4]
                P.op("pe", "matmul", [f"Tg{s2}", "identb"], [GP[j1 // 4]], out=bank[:, (j1 % 4) * 128:(j1 % 4 + 1) * 128],
                     lhsT=Tg[s2][:, j1, :], rhs=identb[:], start=(h == 0 and j1 % 4 == 0), stop=(h == 7 and j1 % 4 == 3))
            if h == 7:
                gs = c16 % 2
                for hb2 in range(2):
                    P.op("act", "copy", [GP[hb2]], [f"gts{gs}"], out=gts[gs][:, hb2 * 4:(hb2 + 1) * 4, :],
                         in_=gtp[gp + hb2][:].rearrange("p (a b) -> p a b", a=4))
                P.dma("pool", f"b_gts{gs}", GTd[c16 * 8:(c16 + 1) * 8, :, j * 128:(j + 1) * 128].rearrange("b e t -> e b t"),
                      gts[gs][:], r=[f"gts{gs}"], w=["GTd"])

        for i in range(129):
            if i < 128:
                G1(i)
            if i >= 1:
                G2(i - 1)
    P.barrier()
    ar.reset()

    Y2 = ar.alloc([128, NTB, 1024], F32)
    CH = 4
    dT = [ar.alloc([128, CH * 8, 128], BF16) for i in range(2)]
    uB = [ar.alloc([128, CH, 1024], BF16) for i in range(2)]
    dst = [ar.alloc([128, 1024], F32) for i in range(2)]
    ust = [ar.alloc([128, 1024], F32) for i in range(2)]
    dbf = [ar.alloc([128, 1024], BF16) for i in range(2)]
    gtb = [ar.alloc([128, CH, 256], BF16) for i in range(2)]
    gl = [ar.alloc([128, 256], F32) for i in range(2)]
    actT = [ar.alloc([128, 256], BF16) for i in range(2)]
    a_ps = mm
    y2_ps = gtp
    down, up = io["peer_down"], io["peer_up"]
    NCH = 128 // CH

    def prep(eb):
        ch, ebi = eb // CH, eb % CH
        cs = ch % 2
        s = eb % 2
        P.dma("sp", f"b_dst{s}", dst[s][:], down[eb * 128:(eb + 1) * 128, :], w=[f"dst{s}"])
        P.dma("sp", f"b_ust{s}", ust[s][:], up[eb * 128:(eb + 1) * 128, :], w=[f"ust{s}"])
        P.op("pool", "tensor_copy", [f"dst{s}"], [f"dbf{s}"], out=dbf[s][:], in_=dst[s][:])
        for kk in range(8):
            P.op("pe", "transpose", [f"dbf{s}", "identb"], ["trb"], out=trb[:, kk * 128:(kk + 1) * 128],
                 in_=dbf[s][:, kk * 128:(kk + 1) * 128], identity=identb[:])
        P.op("act", "copy", ["trb"], [f"dT{cs}"], out=dT[cs][:, ebi * 8:(ebi + 1) * 8, :],
             in_=trb[:].rearrange("p (a b) -> p a b", a=8))
        P.op("pool", "tensor_copy", [f"ust{s}"], [f"uB{cs}"], out=uB[cs][:, ebi, :], in_=ust[s][:])

    mjobs = [(ch, g, ebi) for ch in range(NCH) for g in range(NG) for ebi in range(CH)]

    def M1(i):
        ch, g, ebi = mjobs[i]
        cs, es = ch % 2, i % 2
        gs = (ch * NG + g) % 2
        if ebi == 1 and ch + 1 < NCH:
            for e in range(g * CH // NG, (g + 1) * CH // NG):
                prep((ch + 1) * CH + e)
        if ebi == 0:
            P.dma("sp", f"b_gtb{gs}", gtb[gs][:],
                  GTd[ch * CH:(ch + 1) * CH, :, g * 256:(g + 1) * 256].rearrange("b e t -> e b t"), r=["GTd"], w=[f"gtb{gs}"])
        for kk in range(8):
            P.op("pe", "matmul", [f"dT{cs}", "h2T"], [f"mm{es}"], out=a_ps[es][:, 0:256], lhsT=dT[cs][:, ebi * 8 + kk, :],
                 rhs=h2T[:, kk, g * 256:(g + 1) * 256], start=(kk == 0), stop=(kk == 7))
        P.op("act", "activation", [f"mm{es}"], [f"gl{es}"], out=gl[es][:], in_=a_ps[es][:, 0:256], func=AF.Gelu)
        P.op("dve", "tensor_tensor", [f"gl{es}", f"gtb{gs}"], [f"actT{es}"], out=actT[es][:], in0=gl[es][:],
             in1=gtb[gs][:, ebi, :], op=ALU.mult)

    def M2(i):
        ch, g, ebi = mjobs[i]
        cs, es = ch % 2, i % 2
        for tt in range(2):
            for dh in range(2):
                P.op("pe", "matmul", [f"actT{es}", f"uB{cs}"], [f"gtp{tt * 2 + dh}"], out=y2_ps[tt * 2 + dh][:],
                     lhsT=actT[es][:, tt * 128:(tt + 1) * 128], rhs=uB[cs][:, ebi, dh * 512:(dh + 1) * 512],
                     start=(ebi == 0), stop=(ebi == CH - 1))
        if ebi == CH - 1:
            for tt in range(2):
                for dh in range(2):
                    dstv = Y2[:, g * 2 + tt, dh * 512:(dh + 1) * 512]
                    if ch == 0:
                        P.op("dve", "tensor_copy", [f"gtp{tt * 2 + dh}"], ["Y2"], out=dstv, in_=y2_ps[tt * 2 + dh][:])
                    else:
                        P.op("dve", "tensor_tensor", [f"gtp{tt * 2 + dh}", "Y2"], ["Y2"], out=dstv, in0=y2_ps[tt * 2 + dh][:],
                             in1=dstv, op=ALU.add)

    for e in range(CH):
        prep(e)
    for i in range(len(mjobs) + 1):
        if i < len(mjobs):
            M1(i)
        if i >= 1:
            M2(i - 1)
    xf, of = dst, ust
    for j in range(NTB):
        s = j % 2
        P.dma("sp", f"b_dst{s}", xf[s][:], x1d[j * 128:(j + 1) * 128, :], r=["x1d"], w=[f"dst{s}"])
        P.op("dve", "tensor_tensor", ["Y2", "modB"], [f"ust{s}"], out=of[s][:], in0=Y2[:, j, :], in1=gate2, op=ALU.mult)
        P.op("pool", "tensor_tensor", [f"ust{s}", f"dst{s}"], [f"ust{s}"], out=of[s][:], in0=of[s][:], in1=xf[s][:], op=ALU.add)
        P.dma("pool", f"b_out{s}", out[j * 128:(j + 1) * 128, :], of[s][:], r=[f"ust{s}"], w=[])
    P.barrier()


def build_b(T):
    nc = bass.Bass("TRN2", target_bir_lowering=False)

    def din(name, shape, dt=F32):
        return nc.dram_tensor(name, list(shape), dt, kind="ExternalInput").ap()

    io = dict(
        yg=din("yg", [T, 1024], BF16), xs=din("xs", [T, 1024]), cT=din("cT", [128, 8]),
        ada_w=din("ada_w", [1024, 6144]), ada_b=din("ada_b", [1, 6144]), g2=din("g2", [1, 1024]),
        w_out=din("w_out", [1024, 1024]), w_query=din("w_query", [1024, 2048]), sub_keys=din("sub_keys", [16, 128, 128]),
        peer_down=din("peer_down", [16384, 1024]), peer_up=din("peer_up", [16384, 1024]), ident=din("ident", [128, 128]),
    )
    out = nc.dram_tensor("out", [T, 1024], F32, kind="ExternalOutput").ap()
    with ExitStack() as st:
        P = Prog(nc)
        k = K(nc, P, st)

        def load_y(j, dst, key):
            P.dma("sp", key, dst[:], io["yg"][j * 128:(j + 1) * 128, :], w=[key])

        emit_phase_b(k, io, T, load_y, out)
        P.emit(st)
    return nc


def phase_b_inputs(T, Yrev, xrev, c, ada_w, ada_b, norm2_gain, w_out, peer_w_query, peer_sub_keys, peer_down, peer_up):
    cT = np.ascontiguousarray(c[0].reshape(8, 128).T)
    ncore = Yrev.shape[0] // T
    maps = []
    for ci in range(ncore):
        maps.append(dict(
            yg=np.ascontiguousarray(Yrev[ci * T:(ci + 1) * T]), xs=np.ascontiguousarray(xrev[ci * T:(ci + 1) * T]), cT=cT,
            ada_w=np.ascontiguousarray(ada_w[0]), ada_b=np.ascontiguousarray(ada_b), g2=np.ascontiguousarray(norm2_gain),
            w_out=np.ascontiguousarray(w_out[0]), w_query=np.ascontiguousarray(peer_w_query[0]),
            sub_keys=np.ascontiguousarray(peer_sub_keys[0].reshape(16, 128, 128)),
            peer_down=np.ascontiguousarray(peer_down[0]), peer_up=np.ascontiguousarray(peer_up[0]),
            ident=np.eye(128, dtype=np.float32)))
    return maps


def build_a(S):
    nc = bass.Bass("TRN2", target_bir_lowering=False)
    NT = S // 128

    def din(name, shape, dt=F32):
        return nc.dram_tensor(name, list(shape), dt, kind="ExternalInput").ap()

    io = dict(
        xr=din("xr", [S, 1024]), posT=din("posT", [128, NT], I32), cT=din("cT", [128, 8]),
        ada_w=din("ada_w", [1024, 6144]), ada_b=din("ada_b", [1, 6144]), g1=din("g1", [1, 1024]),
        w_c=din("w_c", [1024, 448]), gains4=din("gains4", [1, 256]),
        DT=din("DT", [128, 128]), xi_b=din("xi_b", [64, 128]), zeta8=din("zeta8", [128, 1]), cd=din("cd", [64, 1]),
        invf=din("invf", [128, 32]), sbmask=din("sbmask", [128, 128]), ident=din("ident", [128, 128]),
    )
    yout = nc.dram_tensor("yout", [S, 128], BF16, kind="ExternalOutput").ap()
    with ExitStack() as st:
        P = Prog(nc)
        k = K(nc, P, st)
        emit_phase_a(k, io, S, yout)
        P.emit(st)
    return nc


def phase_a_inputs(S, x, c, positions, ada_w, ada_b, norm1_gain, w_in, ret_norm_gain, sb_q_gain, sb_k_gain, sb_out_gain):
    NT = S // 128
    xr = np.ascontiguousarray(x[0, ::-1, :])
    posr = np.ascontiguousarray(positions[0, ::-1]).astype(np.int32)
    posT = np.ascontiguousarray(posr.reshape(NT, 128).T)
    cT = np.ascontiguousarray(c[0].reshape(8, 128).T)
    maps = []
    for h in range(NCORES):
        cols = []
        for base in (0, 512, 1024, 1536, 2048, 2560, 3072):
            cols.extend(range(base + h * 64, base + (h + 1) * 64))
        w_c = np.ascontiguousarray(w_in[0][:, cols])
        gains4 = np.concatenate([ret_norm_gain[0, h * 64:(h + 1) * 64], sb_q_gain[0], sb_k_gain[0], sb_out_gain[0]])[None, :]
        m = dict(xr=xr, posT=posT, cT=cT, ada_w=np.ascontiguousarray(ada_w[0]), ada_b=np.ascontiguousarray(ada_b),
                 g1=np.ascontiguousarray(norm1_gain), w_c=w_c, gains4=np.ascontiguousarray(gains4.astype(np.float32)))
        m.update(build_consts(h))
        maps.append(m)
    return maps


def run_phase_a(S, **inp):
    nc = build_a(S)
    maps = phase_a_inputs(S, inp["x"], inp["c"], inp["positions"], inp["ada_w"], inp["ada_b"], inp["norm1_gain"], inp["w_in"],
                          inp["ret_norm_gain"], inp["sb_q_gain"], inp["sb_k_gain"], inp["sb_out_gain"])
    res = run_bass_kernel_spmd(nc, maps, core_ids=list(range(NCORES)))
    ys = [np.asarray(r["yout"]) for r in res.results]
    return np.concatenate(ys, axis=1)


def build_fused(S):
    nc = bass.Bass("TRN2", target_bir_lowering=False)
    NT = S // 128
    T = S // NCORES
    NTB = T // 128

    def din(name, shape, dt=F32):
        return nc.dram_tensor(name, list(shape), dt, kind="ExternalInput").ap()

    io = dict(
        xr=din("xr", [S, 1024]), posT=din("posT", [128, NT], I32), cT=din("cT", [128, 8]),
        ada_w=din("ada_w", [1024, 6144]), ada_b=din("ada_b", [1, 6144]), g1=din("g1", [1, 1024]),
        w_c=din("w_c", [1024, 448]), gains4=din("gains4", [1, 256]),
        DT=din("DT", [128, 128]), xi_b=din("xi_b", [64, 128]), zeta8=din("zeta8", [128, 1]), cd=din("cd", [64, 1]),
        invf=din("invf", [128, 32]), sbmask=din("sbmask", [128, 128]), ident=din("ident", [128, 128]),
        xs=din("xs", [T, 1024]), g2=din("g2", [1, 1024]),
        w_out=din("w_out", [1024, 1024]), w_query=din("w_query", [1024, 2048]), sub_keys=din("sub_keys", [16, 128, 128]),
        peer_down=din("peer_down", [16384, 1024]), peer_up=din("peer_up", [16384, 1024]),
        yidx=din("yidx", [128, NTB * 8], I32),
    )
    out = nc.dram_tensor("out", [T, 1024], F32, kind="ExternalOutput").ap()
    Yb = nc.dram_tensor("Yb", [S, 128], BF16)
    AG = nc.dram_tensor("AG", [NCORES * S, 128], BF16)
    with ExitStack() as st:
        P = Prog(nc)
        k = K(nc, P, st)
        emit_phase_a(k, io, S, Yb.ap())
        P.dma("pool", "cc", None, None, r=[], w=["AG"], meth="collective_compute", inc=0,
              kind="AllGather", op=ALU.bypass, replica_groups=[list(range(NCORES))],
              ins=[Yb.ap().opt()], outs=[AG.ap().opt()])
        P.barrier()
        k.reset()
        yidx = k.sb("yidx", [128, NTB * 8], I32)
        P.dma("sp", "yidx", yidx[:], io["yidx"], w=["yidx"])
        AGap = AG.ap()

        def load_y(j, dst, key):
            for r in range(NCORES):
                P.dma("pool", key, dst[:, r * 128:(r + 1) * 128], AGap, r=["AG", "yidx"], w=[key], meth="indirect_dma_start",
                      out_offset=None, in_offset=bass.IndirectOffsetOnAxis(ap=yidx[:, j * 8 + r: j * 8 + r + 1], axis=0))

        emit_phase_b(k, io, T, load_y, out)
        P.emit(st)
    return nc


def fused_inputs(S, inputs):
    T = S // NCORES
    NTB = T // 128
    ma = phase_a_inputs(S, inputs["x"], inputs["c"], inputs["positions"], inputs["ada_w"], inputs["ada_b"], inputs["norm1_gain"],
                        inputs["w_in"], inputs["ret_norm_gain"], inputs["sb_q_gain"], inputs["sb_k_gain"], inputs["sb_out_gain"])
    xrev = ma[0]["xr"]
    p = np.arange(128)[:, None]
    maps = []
    for ci in range(NCORES):
        m = dict(ma[ci])
        m["xs"] = np.ascontiguousarray(xrev[ci * T:(ci + 1) * T])
        m["g2"] = np.ascontiguousarray(inputs["norm2_gain"])
        m["w_out"] = np.ascontiguousarray(inputs["w_out"][0])
        m["w_query"] = np.ascontiguousarray(inputs["peer_w_query"][0])
        m["sub_keys"] = np.ascontiguousarray(inputs["peer_sub_keys"][0].reshape(16, 128, 128))
        m["peer_down"] = np.ascontiguousarray(inputs["peer_down"][0])
        m["peer_up"] = np.ascontiguousarray(inputs["peer_up"][0])
        jr = np.arange(NTB * 8)[None, :]
        j, r = jr // 8, jr % 8
        m["yidx"] = np.ascontiguousarray((r * S + ci * T + j * 128 + p).astype(np.int32))
        maps.append(m)
    return maps


def run_fused(S, inputs):
    nc = build_fused(S)
    maps = fused_inputs(S, inputs)
    res = run_bass_kernel_spmd(nc, maps, core_ids=list(range(NCORES)))
    out_rev = np.concatenate([np.asarray(r["out"]) for r in res.results], axis=0)
    return np.ascontiguousarray(out_rev[::-1])[None].astype(np.float32)


def kernel(**inputs):
    inputs = {k: np.asarray(v) for k, v in inputs.items()}
    return run_fused(SEQ, inputs)
```
